# Optimizing a Trainium2 kernel written in Bass

```python
import jax, jax.numpy as jnp
from jax import lax
import numpy as np

D_MODEL = 1024
BATCH = 2
SEQ = 16384
DEPTH = 2

MIX_WIDTH = 2 * D_MODEL
HEAD_DIM = 128
HGRN_WIDTH = MIX_WIDTH // 2
NSA_WIDTH = MIX_WIDTH - HGRN_WIDTH
HGRN_HEADS = HGRN_WIDTH // HEAD_DIM
NSA_HEADS = NSA_WIDTH // HEAD_DIM
NSA_KV_GROUPS = 2
NSA_HEADS_PER_GROUP = NSA_HEADS // NSA_KV_GROUPS
KV_WIDTH = NSA_KV_GROUPS * HEAD_DIM
N_BRANCHES = 3
HGRN_CHUNK = 64
CMP_BLOCK = 32
CMP_STRIDE = 16
SEL_BLOCK = 64
N_SELECT = 16
WINDOW = 512
Q_BLOCK = 128
ROPE_THETA = 500000.0
ROT_DIM = HEAD_DIM // 4
NORM_EPS = 1e-6
FORCE_SCORE = 1e4
IN_SPLITS = (HGRN_WIDTH, HGRN_WIDTH, HGRN_WIDTH, HGRN_WIDTH,
             NSA_WIDTH, KV_WIDTH, KV_WIDTH, KV_WIDTH, KV_WIDTH, KV_WIDTH, KV_WIDTH,
             NSA_HEADS * N_BRANCHES, NSA_WIDTH)
IN_COLS = sum(IN_SPLITS)

kernel_name = "hgrn2_nsa_parallel_hybrid"


def rmsnorm(x, g):
    xf = x.astype(jnp.float32)
    y = xf * lax.rsqrt(jnp.mean(xf * xf, axis=-1, keepdims=True) + NORM_EPS)
    return (y * g.astype(jnp.float32)).astype(x.dtype)


def partial_rope(x, pos):
    half = ROT_DIM // 2
    inv = ROPE_THETA ** (-2.0 * jnp.arange(half, dtype=jnp.float32) / ROT_DIM)
    ang = pos.astype(jnp.float32)[:, None] * inv[None, :]
    cos = jnp.cos(ang)[None, :, None, :]
    sin = jnp.sin(ang)[None, :, None, :]
    xf = x.astype(jnp.float32)
    x1 = xf[..., :half]
    x2 = xf[..., half:ROT_DIM]
    out = jnp.concatenate([x1 * cos - x2 * sin, x2 * cos + x1 * sin, xf[..., ROT_DIM:]], axis=-1)
    return out.astype(x.dtype)


def masked_softmax(s, mask, axis):
    s = jnp.where(mask, s, -jnp.inf)
    m = jnp.max(s, axis=axis, keepdims=True)
    m = jnp.where(jnp.isfinite(m), m, 0.0)
    p = jnp.exp(s - m)
    return p / jnp.maximum(jnp.sum(p, axis=axis, keepdims=True), 1e-30)


def hgrn2_mixer(q, f_logit, i, lb):
    B, S, H, D = q.shape
    C = HGRN_CHUNK
    N = S // C
    log_f = jnp.logaddexp(jnp.log(lb), jnp.log1p(-lb) + jax.nn.log_sigmoid(f_logit.astype(jnp.float32)))
    k = -jnp.expm1(log_f)

    def chunks(a):
        return a.astype(jnp.float32).reshape(B, N, C, H, D).transpose(1, 0, 3, 2, 4)

    qc, kc, vc = chunks(q), chunks(k), chunks(i)
    bc = jnp.cumsum(chunks(log_f), axis=3)
    causal = jnp.tril(jnp.ones((C, C), dtype=bool))

    def step(state, xs):
        q_, k_, v_, b_ = xs
        o_inter = jnp.einsum('bhtk,bhkv->bhtv', q_ * jnp.exp(b_), state)
        diff = jnp.where(causal[:, :, None], b_[:, :, :, None, :] - b_[:, :, None, :, :], -jnp.inf)
        attn = jnp.einsum('bhtk,bhsk,bhtsk->bhts', q_, k_, jnp.exp(diff))
        o_intra = jnp.einsum('bhts,bhsv->bhtv', attn, v_)
        b_last = b_[:, :, -1:, :]
        state = state * jnp.exp(b_last[:, :, 0, :, None]) + jnp.einsum(
            'bhsk,bhsv->bhkv', k_ * jnp.exp(b_last - b_), v_)
        return state, o_inter + o_intra

    s0 = jnp.zeros((B, H, D, D), jnp.float32)
    _, o = lax.scan(step, s0, (qc, kc, vc, bc))
    return o.transpose(1, 0, 3, 2, 4).reshape(B, S, H, D).astype(q.dtype)


def compress(kv, pos_emb, w1, b1, w2):
    B, S, G, D = kv.shape
    n_cmp = (S - CMP_BLOCK) // CMP_STRIDE + 1
    idx = jnp.arange(n_cmp)[:, None] * CMP_STRIDE + jnp.arange(CMP_BLOCK)[None, :]
    blocks = kv[:, idx] + pos_emb[None, None, :, None, :]
    flat = blocks.transpose(0, 1, 3, 2, 4).reshape(B, n_cmp, G, CMP_BLOCK * D)
    return jax.nn.silu(flat @ w1 + b1) @ w2


def nsa_mixer(q, kc, vc, ks, vs, kw, vw, gate_logits, cmp_k, cmp_v):
    B, S, H, D = q.shape
    G, HG = NSA_KV_GROUPS, NSA_HEADS_PER_GROUP
    pos = jnp.arange(S)
    q = partial_rope(q, pos)
    ks = partial_rope(ks, pos)
    kw = partial_rope(kw, pos)
    k_cmp = compress(kc, *cmp_k)
    v_cmp = compress(vc, *cmp_v).astype(jnp.float32)
    n_cmp = k_cmp.shape[1]
    cmp_start = jnp.arange(n_cmp) * CMP_STRIDE
    cmp_end = cmp_start + CMP_BLOCK - 1
    k_cmp = partial_rope(k_cmp, cmp_end).astype(jnp.float32)
    n_sel_blocks = S // SEL_BLOCK
    n_top = min(N_SELECT, n_sel_blocks)
    sel_start = jnp.arange(n_sel_blocks) * SEL_BLOCK
    overlap = ((cmp_start[:, None] < sel_start[None, :] + SEL_BLOCK)
               & (cmp_start[:, None] + CMP_BLOCK > sel_start[None, :])).astype(jnp.float32)
    ks_blocks = ks.reshape(B, n_sel_blocks, SEL_BLOCK, G, D).transpose(0, 3, 1, 2, 4)
    vs_blocks = vs.reshape(B, n_sel_blocks, SEL_BLOCK, G, D).transpose(0, 3, 1, 2, 4)
    kw_pad = jnp.pad(kw, ((0, 0), (WINDOW, 0), (0, 0), (0, 0)))
    vw_pad = jnp.pad(vw, ((0, 0), (WINDOW, 0), (0, 0), (0, 0)))
    gates = jax.nn.sigmoid(gate_logits.astype(jnp.float32)).reshape(B, S, G, HG, N_BRANCHES)
    scale = HEAD_DIM ** -0.5
    b_idx = jnp.arange(B)[:, None, None, None]
    g_idx = jnp.arange(G)[None, :, None, None]
    blk = jnp.arange(n_sel_blocks)

    def query_block(qb):
        q0 = qb * Q_BLOCK
        t = q0 + jnp.arange(Q_BLOCK)
        qblk = lax.dynamic_slice_in_dim(q, q0, Q_BLOCK, axis=1).reshape(
            B, Q_BLOCK, G, HG, D).astype(jnp.float32)
        s_c = jnp.einsum('btghd,bngd->bghtn', qblk, k_cmp) * scale
        p_c = masked_softmax(s_c, cmp_end[None, :] <= t[:, None], -1)
        o_c = jnp.einsum('bghtn,bngd->btghd', p_c, v_cmp)
        imp = jnp.einsum('bghtn,nj->bgtj', p_c, overlap)
        cur = t // SEL_BLOCK
        forced = (blk[None, :] == 0) | (blk[None, :] == cur[:, None]) | (blk[None, :] == cur[:, None] - 1)
        valid = blk[None, :] * SEL_BLOCK <= t[:, None]
        imp = jnp.where(forced, FORCE_SCORE, jnp.where(valid, imp, -1.0))
        _, top = lax.top_k(imp, n_top)
        k_s = ks_blocks[b_idx, g_idx, top].astype(jnp.float32)
        v_s = vs_blocks[b_idx, g_idx, top].astype(jnp.float32)
        key_pos = top[..., None] * SEL_BLOCK + jnp.arange(SEL_BLOCK)
        mask_s = (key_pos <= t[None, None, :, None, None])[:, :, None]
        s_s = jnp.einsum('btghd,bgtnld->bghtnl', qblk, k_s) * scale
        p_s = masked_softmax(s_s, mask_s, (-2, -1))
        o_s = jnp.einsum('bghtnl,bgtnld->btghd', p_s, v_s)
        k_w = lax.dynamic_slice_in_dim(kw_pad, q0, WINDOW + Q_BLOCK, axis=1).astype(jnp.float32)
        v_w = lax.dynamic_slice_in_dim(vw_pad, q0, WINDOW + Q_BLOCK, axis=1).astype(jnp.float32)
        kp = q0 - WINDOW + jnp.arange(WINDOW + Q_BLOCK)
        mask_w = (kp[None, :] >= 0) & (kp[None, :] <= t[:, None]) & (t[:, None] - kp[None, :] < WINDOW)
        s_w = jnp.einsum('btghd,bkgd->bghtk', qblk, k_w) * scale
        p_w = masked_softmax(s_w, mask_w, -1)
        o_w = jnp.einsum('bghtk,bkgd->btghd', p_w, v_w)
        g = lax.dynamic_slice_in_dim(gates, q0, Q_BLOCK, axis=1)
        o = g[..., 0:1] * o_c + g[..., 1:2] * o_s + g[..., 2:3] * o_w
        return o.reshape(B, Q_BLOCK, H, D)

    out = lax.map(query_block, jnp.arange(S // Q_BLOCK))
    return out.transpose(1, 0, 2, 3, 4).reshape(B, S, H, D).astype(q.dtype)


def setup_inputs(seed: int = 0) -> dict:
    key = jax.random.key(seed)
    k = jax.random.split(key, 16)

    def nrm(kk, shape, scale):
        return jax.random.normal(kk, shape, jnp.float32) * scale

    L, D = CMP_BLOCK, HEAD_DIM
    return {
        "x": nrm(k[0], (BATCH, SEQ, D_MODEL), 1.0),
        "pre_norm": 1.0 + nrm(k[1], (DEPTH, D_MODEL), 0.02),
        "post_norm": 1.0 + nrm(k[2], (DEPTH, D_MODEL), 0.02),
        "w_in": nrm(k[3], (DEPTH, D_MODEL, IN_COLS), D_MODEL ** -0.5),
        "hgrn_lb_logits": nrm(k[4], (DEPTH, HGRN_WIDTH), 0.5),
        "hgrn_out_norm": 1.0 + nrm(k[5], (DEPTH, HEAD_DIM), 0.02),
        "cmp_pos_k": nrm(k[6], (DEPTH, L, D), 0.1),
        "cmp_w1_k": nrm(k[7], (DEPTH, L * D, D), (L * D) ** -0.5),
        "cmp_b1_k": nrm(k[8], (DEPTH, D), 0.01),
        "cmp_w2_k": nrm(k[9], (DEPTH, D, D), D ** -0.5),
        "cmp_pos_v": nrm(k[10], (DEPTH, L, D), 0.1),
        "cmp_w1_v": nrm(k[11], (DEPTH, L * D, D), (L * D) ** -0.5),
        "cmp_b1_v": nrm(k[12], (DEPTH, D), 0.01),
        "cmp_w2_v": nrm(k[13], (DEPTH, D, D), D ** -0.5),
        "w_out": nrm(k[14], (DEPTH, MIX_WIDTH, D_MODEL), MIX_WIDTH ** -0.5),
    }


def reference(x, pre_norm, post_norm, w_in, hgrn_lb_logits, hgrn_out_norm,
              cmp_pos_k, cmp_w1_k, cmp_b1_k, cmp_w2_k,
              cmp_pos_v, cmp_w1_v, cmp_b1_v, cmp_w2_v, w_out):
    B, S, _ = x.shape
    lb_probs = jax.nn.softmax(hgrn_lb_logits.astype(jnp.float32), axis=0)
    lower_bounds = jnp.maximum(jnp.cumsum(lb_probs, axis=0) - lb_probs[0:1], 0.0)
    offsets = np.cumsum(IN_SPLITS)[:-1].tolist()

    def heads(a):
        return a.reshape(B, S, -1, HEAD_DIM)

    for layer in range(DEPTH):
        h = rmsnorm(x, pre_norm[layer])
        proj = h @ w_in[layer]
        (hq, hf, hi, hz, nq, kc, vc, ks, vs, kw, vw, ng, nz) = jnp.split(proj, offsets, axis=-1)
        o_h = hgrn2_mixer(heads(hq), heads(hf), heads(hi),
                          lower_bounds[layer].reshape(HGRN_HEADS, HEAD_DIM))
        o_h = rmsnorm(o_h, hgrn_out_norm[layer]).reshape(B, S, HGRN_WIDTH) * jax.nn.silu(hz)
        o_n = nsa_mixer(heads(nq), heads(kc), heads(vc), heads(ks), heads(vs), heads(kw), heads(vw), ng,
                        (cmp_pos_k[layer], cmp_w1_k[layer], cmp_b1_k[layer], cmp_w2_k[layer]),
                        (cmp_pos_v[layer], cmp_w1_v[layer], cmp_b1_v[layer], cmp_w2_v[layer]))
        o_n = o_n.reshape(B, S, NSA_WIDTH) * jax.nn.silu(nz)
        y = jnp.concatenate([o_h, o_n], axis=-1).astype(x.dtype) @ w_out[layer]
        x = x + rmsnorm(y, post_norm[layer]).astype(x.dtype)
    return x
```

```python
import math
import numpy as np
from contextlib import ExitStack
import concourse.bass as bass
import concourse.mybir as mybir
from concourse.bass_utils import run_bass_kernel_spmd

F32 = mybir.dt.float32
BF16 = mybir.dt.bfloat16
AF = mybir.ActivationFunctionType
ALU = mybir.AluOpType
AX = mybir.AxisListType

D_MODEL = 1024
SEQ = 16384
HD = 128
EPS = 1e-6
ROPE_THETA = 500000.0
SCALE = HD ** -0.5
NEG = -3.0e38


class Buf:
    def __init__(self, t, name):
        self.t = t
        self.name = name
        self.lw = None
        self.rd = []

    def __getitem__(self, k):
        return self.t[k]


class FW:
    def __init__(self, nc, es, n_dma_sems=32):
        self.nc = nc
        self.es = es
        self.eng = {"pe": nc.tensor, "act": nc.scalar, "dve": nc.vector, "pool": nc.gpsimd, "sp": nc.sync}
        self.sem = {}
        self.cnt = {}
        for e in ("pe", "act", "dve", "pool"):
            self.sem[e] = es.enter_context(nc.semaphore("sem_" + e))
            self.cnt[e] = 0
        self.dsem = [es.enter_context(nc.semaphore("dsem%d" % i)) for i in range(n_dma_sems)]
        self.dcnt = [0] * n_dma_sems
        self.dnext = 0
        self.waited = {e: {} for e in self.eng}
        self.nbuf = 0

    def sb(self, shape, dt, name=None):
        self.nbuf += 1
        name = name or ("sb%d" % self.nbuf)
        return Buf(self.es.enter_context(self.nc.sbuf_tensor(name, list(shape), dt)), name)

    def ps(self, shape, dt, name=None):
        self.nbuf += 1
        name = name or ("ps%d" % self.nbuf)
        return Buf(self.es.enter_context(self.nc.psum_tensor(name, list(shape), dt)), name)

    def dram(self, name, shape, dt, kind="Internal"):
        t = self.nc.dram_tensor(name, list(shape), dt, kind=kind)
        return Buf(t.ap(), name)

    def _semobj(self, key):
        return self.sem[key] if isinstance(key, str) else self.dsem[key]

    def _wait(self, e, ticket):
        if ticket is None:
            return
        key, val = ticket
        if key == e and e == "pe":
            return
        w = self.waited[e]
        if w.get(key, 0) >= val:
            return
        self.eng[e].wait_ge(self._semobj(key), val)
        w[key] = val

    def _deps(self, e, reads, writes):
        for b in reads:
            self._wait(e, b.lw)
        for b in writes:
            self._wait(e, b.lw)
            for t in b.rd:
                self._wait(e, t)

    def _commit(self, ticket, reads, writes):
        for b in reads:
            b.rd.append(ticket)
            if len(b.rd) > 32:
                best = {}
                for k, v in b.rd:
                    if best.get(k, 0) < v:
                        best[k] = v
                b.rd = list(best.items())
        for b in writes:
            b.lw = ticket
            b.rd = []

    def op(self, e, fn, reads=(), writes=(), signal=True):
        self._deps(e, reads, writes)
        ins = fn(self.eng[e])
        if signal:
            self.cnt[e] += 1
            ins.then_inc(self.sem[e], 1)
            t = (e, self.cnt[e])
        else:
            t = (e, self.cnt[e] + 1)
        assert self.cnt[e] < 4000000
        self._commit(t, reads, writes)
        return t

    def dma(self, e, out_ap, in_ap, reads=(), writes=(), **kw):
        s = self.dnext
        self.dnext = (self.dnext + 1) % len(self.dsem)
        if self.dcnt[s] > 0:
            self._wait(e, (s, self.dcnt[s]))
        self._deps(e, reads, writes)
        ins = self.eng[e].dma_start(out=out_ap, in_=in_ap, **kw)
        self.dcnt[s] += 16
        ins.then_inc(self.dsem[s], 16)
        t = (s, self.dcnt[s])
        self._commit(t, reads, writes)
        return t

    def barrier(self):
        for e in self.eng:
            for k in ("pe", "act", "dve", "pool"):
                if self.cnt[k] > 0:
                    self._wait_force(e, (k, self.cnt[k]))
            for s_, c in enumerate(self.dcnt):
                if c > 0:
                    self._wait_force(e, (s_, c))

    def _wait_force(self, e, ticket):
        key, val = ticket
        if key == e:
            return
        w = self.waited[e]
        if w.get(key, 0) >= val:
            return
        self.eng[e].wait_ge(self._semobj(key), val)
        w[key] = val

    def finish(self, bufs, e="sp"):
        for b in bufs:
            self._wait(e, b.lw)


class Rot:
    def __init__(self, bufs):
        self.bufs = bufs
        self.i = 0

    def next(self):
        b = self.bufs[self.i % len(self.bufs)]
        self.i += 1
        return b


FM_BLOCKS = ["hq0", "hq1", "hf0", "hf1", "nq0", "nq1", "nq2", "nq3", "nq0s", "nq1s", "nq2s", "nq3s",
             "kc", "vc", "ks", "kss", "kw", "kws"]
NFM = len(FM_BLOCKS)
FMI = {n: i for i, n in enumerate(FM_BLOCKS)}
FMS = ["hq0", "hq1", "kk0", "kk1", "nq0", "nq1", "nq2", "nq3", "kc", "vc", "ks", "kw"]
FMSI = {n: i for i, n in enumerate(FMS)}
NTM = 1024 + 6


def build_mixer(S, phases=(1, 2, 3, 4), debug=()):
    nc = bass.Bass("TRN2", target_bir_lowering=False)
    NT5 = S // 512
    NQT = S // 128
    NCMP = S // 16 - 1
    NCT = (NCMP + 127) // 128
    NCP = NCT * 128

    def din(name, shape, dt=F32):
        return Buf(nc.dram_tensor(name, list(shape), dt, kind="ExternalInput").ap(), name)

    def dout(name, shape, dt=F32):
        return Buf(nc.dram_tensor(name, list(shape), dt, kind="ExternalOutput").ap(), name)

    xT = din("xT", [1024, S])
    gpre = din("gpre", [128, 8])
    wfm = din("wfm", [1024, NFM * 128])
    wtm = din("wtm", [1024, NTM])
    cosT = din("cosT", [128, S])
    sinT = din("sinT", [128, S])
    lbl = din("lbl", [128, 5])
    gh = din("gh", [128, 128])
    w1k = din("w1k", [128, 32 * 128]); w1v = din("w1v", [128, 32 * 128])
    posk = din("posk", [128, 32]); posv = din("posv", [128, 32])
    b1k = din("b1k", [128, 1]); b1v = din("b1v", [128, 1])
    w2k = din("w2k", [128, 256]); w2v = din("w2v", [128, 128])
    cosC = din("cosC", [128, NCP]); sinC = din("sinC", [128, NCP])
    ovl = din("ovl", [128, NCT * 257])
    e16 = din("e16", [128, 8192])
    segm = din("segm", [128, 512])
    cm64 = din("cm64", [64, 64])
    ident_in = din("ident", [128, 128])
    o_out = dout("o", [S, 512])
    dbg = {}

    with ExitStack() as es:
        fw = FW(nc, es)
        fm_bf = fw.dram("fm_bf", [len(FMS), 128, S], BF16)
        fm_lf = fw.dram("fm_lf", [2, 128, S], F32)
        tm_bf = fw.dram("tm_bf", [S, 1024], BF16)
        tm_ng = fw.dram("tm_ng", [S, 6], F32)
        if debug:
            for nme in debug:
                if nme == "fm_bf":
                    dbg[nme] = dout("d_fm_bf", [len(FMS), 128, S], BF16)
                elif nme == "fm_lf":
                    dbg[nme] = dout("d_fm_lf", [2, 128, S], F32)
                elif nme == "tm_bf":
                    dbg[nme] = dout("d_tm_bf", [S, 1024], BF16)
                elif nme == "tm_ng":
                    dbg[nme] = dout("d_tm_ng", [S, 6], F32)

        FILL0 = nc.gpsimd.to_reg(0.0)
        FILLF = nc.gpsimd.to_reg(1.0e4)
        FILLM = nc.gpsimd.to_reg(-1.0)
        ones_bf = fw.sb([128, 128], BF16, "ones_bf")
        fw.op("pool", lambda e: e.memset(ones_bf[:], 1.0), writes=[ones_bf])
        ident = fw.sb([128, 128], BF16, "ident_sb")
        cst = fw.sb([128, 512], F32, "cst_stage")
        fw.dma("sp", cst[:, 0:128], ident_in[:, :], reads=[ident_in], writes=[cst])
        fw.op("dve", lambda e: e.tensor_copy(out=ident[:], in_=cst[:, 0:128]), reads=[cst], writes=[ident])
        gpre_sb = fw.sb([128, 8], F32, "gpre_sb")
        fw.dma("sp", gpre_sb[:], gpre[:, :], reads=[gpre], writes=[gpre_sb])
        lbl_sb = fw.sb([128, 5], F32, "lbl_sb")
        fw.dma("sp", lbl_sb[:], lbl[:, :], reads=[lbl], writes=[lbl_sb])
        lb_sb = fw.sb([128, 2], F32, "lb_sb")
        oml_sb = fw.sb([128, 2], F32, "oml_sb")
        for h in range(2):
            fw.op("dve", lambda e, h=h: e.tensor_tensor(out=lb_sb[:, h:h + 1], in0=lbl_sb[:, 2 * h + 1:2 * h + 2],
                                                        in1=lbl_sb[:, 2 * h:2 * h + 1], op=ALU.subtract),
                  reads=[lbl_sb], writes=[lb_sb])
        fw.op("act", lambda e: e.activation(out=lb_sb[:], in_=lb_sb[:], func=AF.Sigmoid), reads=[lb_sb], writes=[lb_sb])
        fw.op("dve", lambda e: e.tensor_scalar(out=lb_sb[:], in0=lb_sb[:], scalar1=lbl_sb[:, 4:5], scalar2=None, op0=ALU.mult),
              reads=[lb_sb, lbl_sb], writes=[lb_sb])
        fw.op("dve", lambda e: e.tensor_scalar(out=oml_sb[:], in0=lb_sb[:], scalar1=-1.0, scalar2=1.0, op0=ALU.mult, op1=ALU.add),
              reads=[lb_sb], writes=[oml_sb])

        if 1 in phases:
            with ExitStack() as es1:
                fw.es = es1
                wfm_sb = fw.sb([128, 8, NFM * 128], BF16, "wfm_sb")
                wtm_sb = fw.sb([128, 8, NTM], BF16, "wtm_sb")
                wst = Rot([fw.sb([128, 2048], F32, "wst%d" % i) for i in range(2)])
                wfm_v = wfm[:, :].rearrange("(k p) c -> p k c", p=128)
                wtm_v = wtm[:, :].rearrange("(k p) c -> p k c", p=128)
                ci = 0
                for (src, dst, ncols) in ((wfm_v, wfm_sb, NFM * 128), (wtm_v, wtm_sb, NTM)):
                    for k in range(8):
                        for c0 in range(0, ncols, 2048):
                            cw = min(2048, ncols - c0)
                            st = wst.next()
                            fw.dma("sp", st[:, 0:cw], src[:, k, c0:c0 + cw], reads=[wfm, wtm], writes=[st])
                            eng = "act" if ci % 2 == 0 else "dve"
                            ci += 1
                            if eng == "act":
                                fw.op("act", lambda e, st=st, dst=dst, k=k, c0=c0, cw=cw: e.activation(
                                    out=dst[:, k, c0:c0 + cw], in_=st[:, 0:cw], func=AF.Copy), reads=[st], writes=[dst])
                            else:
                                fw.op("dve", lambda e, st=st, dst=dst, k=k, c0=c0, cw=cw: e.tensor_copy(
                                    out=dst[:, k, c0:c0 + cw], in_=st[:, 0:cw]), reads=[st], writes=[dst])
                xt_r = Rot([fw.sb([128, 8, 512], F32, "xt%d" % i) for i in range(2)])
                sq = fw.sb([128, 8, 512], BF16, "sq")
                hT_r = Rot([fw.sb([128, 8, 512], BF16, "hT%d" % i) for i in range(2)])
                rstd = fw.sb([128, 512], F32, "rstd")
                cs_r = Rot([fw.sb([128, 2, 512], F32, "cs%d" % i) for i in range(2)])
                psb = Rot([fw.ps([128, 512], F32, "p1ps%d" % i) for i in range(6)])
                stg_bf = Rot([fw.sb([128, 512], BF16, "stgbf%d" % i) for i in range(4)])
                stg_f = Rot([fw.sb([128, 512], F32, "stgf%d" % i) for i in range(3)])
                tmp_f = Rot([fw.sb([128, 512], F32, "tmpf%d" % i) for i in range(3)])
                tm_stage = Rot([fw.sb([128, 4, 1024], BF16, "tmst%d" % i) for i in range(2)])
                ng_stage = Rot([fw.sb([128, 4, 6], F32, "ngst%d" % i) for i in range(2)])
                xT_v = xT[:, :].rearrange("(k p) t -> p k t", p=128)

                def fm_mm(ps, blk, hT):
                    for k in range(8):
                        fw.op("pe", lambda e, k=k: e.matmul(ps[:], lhsT=wfm_sb[:, k, blk * 128:(blk + 1) * 128], rhs=hT[:, k, :],
                                                             start=(k == 0), stop=(k == 7)),
                              reads=[wfm_sb, hT], writes=[ps], signal=(k == 7))

                def store_fm(stage, name, ts):
                    fw.dma("pool", fm_bf[FMSI[name], :, ts], stage[:], reads=[stage], writes=[fm_bf])

                for i in range(NT5):
                    ts = slice(i * 512, (i + 1) * 512)
                    xt = xt_r.next()
                    hT = hT_r.next()
                    cs = cs_r.next()
                    fw.dma("sp", xt[:], xT_v[:, :, ts], reads=[xT], writes=[xt])
                    fw.dma("sp", cs[:, 0, :], cosT[:, ts], reads=[cosT], writes=[cs])
                    fw.dma("sp", cs[:, 1, :], sinT[:, ts], reads=[sinT], writes=[cs])
                    fw.op("act", lambda e: e.activation(out=sq[:], in_=xt[:], func=AF.Square), reads=[xt], writes=[sq])
                    ps = psb.next()
                    for k in range(8):
                        fw.op("pe", lambda e, k=k: e.matmul(ps[:], lhsT=ones_bf[:], rhs=sq[:, k, :], start=(k == 0), stop=(k == 7)),
                              reads=[ones_bf, sq], writes=[ps], signal=(k == 7))
                    fw.op("dve", lambda e: e.tensor_scalar(out=rstd[:], in0=ps[:], scalar1=1.0 / 1024, scalar2=EPS, op0=ALU.mult, op1=ALU.add),
                          reads=[ps], writes=[rstd])
                    fw.op("act", lambda e: e.activation(out=rstd[:], in_=rstd[:], func=AF.Sqrt), reads=[rstd], writes=[rstd])
                    fw.op("dve", lambda e: e.reciprocal(out=rstd[:], in_=rstd[:]), reads=[rstd], writes=[rstd])
                    for k in range(8):
                        eng = "dve"
                        fw.op(eng, lambda e, k=k: e.scalar_tensor_tensor(out=hT[:, k, :], in0=xt[:, k, :], scalar=gpre_sb[:, k:k + 1],
                                                                          in1=rstd[:], op0=ALU.mult, op1=ALU.mult),
                              reads=[xt, rstd, gpre_sb], writes=[hT])
                    for name in ("hq0", "hq1", "kc", "vc"):
                        ps = psb.next()
                        fm_mm(ps, FMI[name], hT)
                        st = stg_bf.next()
                        fw.op("act", lambda e: e.activation(out=st[:], in_=ps[:], func=AF.Copy), reads=[ps], writes=[st])
                        store_fm(st, name, ts)
                    for h in range(2):
                        ps = psb.next()
                        fm_mm(ps, FMI["hf%d" % h], hT)
                        sg = tmp_f.next()
                        fw.op("act", lambda e: e.activation(out=sg[:], in_=ps[:], func=AF.Sigmoid), reads=[ps], writes=[sg])
                        fw.op("dve", lambda e: e.tensor_scalar(out=sg[:], in0=sg[:], scalar1=oml_sb[:, h:h + 1], scalar2=lb_sb[:, h:h + 1],
                                                               op0=ALU.mult, op1=ALU.add), reads=[sg, oml_sb, lb_sb], writes=[sg])
                        lf = stg_f.next()
                        fw.op("act", lambda e: e.activation(out=lf[:], in_=sg[:], func=AF.Ln), reads=[sg], writes=[lf])
                        fw.dma("pool", fm_lf[h, :, ts], lf[:], reads=[lf], writes=[fm_lf])
                        sk = tmp_f.next()
                        fw.op("act", lambda e: e.activation(out=sk[:], in_=ps[:], func=AF.Sigmoid, scale=-1.0), reads=[ps], writes=[sk])
                        st = stg_bf.next()
                        fw.op("dve", lambda e: e.tensor_scalar(out=st[:], in0=sk[:], scalar1=oml_sb[:, h:h + 1], scalar2=None, op0=ALU.mult),
                              reads=[sk, oml_sb], writes=[st])
                        store_fm(st, "kk%d" % h, ts)
                    for name in ("nq0", "nq1", "nq2", "nq3", "ks", "kw"):
                        ps = psb.next()
                        fm_mm(ps, FMI[name], hT)
                        ps2 = psb.next()
                        fm_mm(ps2, FMI[name + "s"], hT)
                        t1 = tmp_f.next()
                        fw.op("dve", lambda e: e.tensor_tensor(out=t1[:], in0=ps[:], in1=cs[:, 0, :], op=ALU.mult), reads=[ps, cs], writes=[t1])
                        t2 = tmp_f.next()
                        fw.op("dve", lambda e: e.tensor_tensor(out=t2[:], in0=ps2[:], in1=cs[:, 1, :], op=ALU.mult), reads=[ps2, cs], writes=[t2])
                        st = stg_bf.next()
                        fw.op("pool", lambda e: e.tensor_tensor(out=st[:], in0=t1[:], in1=t2[:], op=ALU.add), reads=[t1, t2], writes=[st])
                        store_fm(st, name, ts)
                    tms = tm_stage.next()
                    ngs = ng_stage.next()
                    for j in range(4):
                        for grp in range(3):
                            c0, cw = ((0, 512), (512, 512), (1024, 6))[grp]
                            ps = psb.next()
                            for k in range(8):
                                fw.op("pe", lambda e, k=k: e.matmul(ps[:, 0:cw], lhsT=hT[:, k, j * 128:(j + 1) * 128], rhs=wtm_sb[:, k, c0:c0 + cw],
                                                                     start=(k == 0), stop=(k == 7)),
                                      reads=[hT, wtm_sb], writes=[ps], signal=(k == 7))
                            if grp < 2:
                                fw.op("act", lambda e: e.activation(out=tms[:, j, c0:c0 + 256], in_=ps[:, 0:256], func=AF.Copy),
                                      reads=[ps], writes=[tms])
                                fw.op("act", lambda e: e.activation(out=tms[:, j, c0 + 256:c0 + 512], in_=ps[:, 256:512], func=AF.Silu),
                                      reads=[ps], writes=[tms])
                            else:
                                fw.op("act", lambda e: e.activation(out=ngs[:, j, :], in_=ps[:, 0:6], func=AF.Sigmoid), reads=[ps], writes=[ngs])
                    fw.dma("pool", tm_bf[ts, :].rearrange("(j p) c -> p j c", p=128), tms[:], reads=[tms], writes=[tm_bf])
                    fw.dma("pool", tm_ng[ts, :].rearrange("(j p) c -> p j c", p=128), ngs[:], reads=[ngs], writes=[tm_ng])
                fw.barrier()
            fw.es = es

        if 2 in phases:
            with ExitStack() as es2:
                fw.es = es2
                segm_sb = fw.sb([128, 512], F32, "segm_sb")
                fw.dma("sp", segm_sb[:], segm[:, :], reads=[segm], writes=[segm_sb])
                cm_sb = fw.sb([64, 64], F32, "cm_sb")
                fw.dma("sp", cm_sb[:], cm64[:, :], reads=[cm64], writes=[cm_sb])
                gh_sb = fw.sb([128, 128], F32, "gh_sb")
                fw.dma("sp", gh_sb[:], gh[:, :], reads=[gh], writes=[gh_sb])
                state = [fw.sb([128, 128], F32, "state%d" % h) for h in range(2)]
                state_bf = [fw.sb([128, 128], BF16, "statebf%d" % h) for h in range(2)]
                for h in range(2):
                    fw.op("pool", lambda e, h=h: e.memset(state[h][:], 0.0), writes=[state[h]])
                    fw.op("pool", lambda e, h=h: e.memset(state_bf[h][:], 0.0), writes=[state_bf[h]])

                def mk(shape, dt, nm):
                    return [Rot([fw.sb(shape, dt, "%s_%d_%d" % (nm, h, i)) for i in range(2)]) for h in range(2)]
                qT_r = mk([128, 512], BF16, "p2q"); kk_r = mk([128, 512], BF16, "p2kk"); lf_r = mk([128, 512], F32, "p2lf")
                v_r = mk([64, 8, 128], BF16, "p2v"); zs_r = mk([64, 8, 128], BF16, "p2zs")
                bT_r = mk([128, 512], F32, "p2b"); d1_r = mk([128, 512], F32, "p2d1"); d4_r = mk([128, 512], F32, "p2d4")
                e1_r = mk([128, 512], F32, "p2e1"); e2_r = mk([128, 512], F32, "p2e2"); e3_r = mk([128, 512], F32, "p2e3"); e4_r = mk([128, 512], F32, "p2e4")
                qe_r = mk([128, 512], BF16, "p2qe"); ke_r = mk([128, 512], BF16, "p2ke"); qb_r = mk([128, 512], BF16, "p2qb"); kd_r = mk([128, 512], BF16, "p2kd")
                osb_r = mk([64, 8, 128], F32, "p2osb"); ofin_r = mk([64, 8, 128], F32, "p2ofin")
                sqt = fw.sb([64, 8, 128], F32, "p2sq")
                ssq = fw.sb([64, 8], F32, "p2ssq")
                kdtm_r = [Rot([fw.sb([64, 128], BF16, "p2kdtm_%d_%d" % (h, i)) for i in range(2)]) for h in range(2)]
                AT_r = [Rot([fw.sb([64, 64], BF16, "p2AT_%d_%d" % (h, i)) for i in range(2)]) for h in range(2)]
                ps_tr = Rot([fw.ps([64, 128], BF16, "p2pstr%d" % i) for i in range(2)])
                ps_at = Rot([fw.ps([64, 64], F32, "p2psat%d" % i) for i in range(2)])
                ps_o = Rot([fw.ps([64, 128], F32, "p2pso%d" % i) for i in range(2)])
                ps_u = Rot([fw.ps([128, 128], F32, "p2psu%d" % i) for i in range(2)])
                for i in range(NT5):
                    ts = slice(i * 512, (i + 1) * 512)
                    T = []
                    for h in range(2):
                        qT_ = qT_r[h].next(); kk_ = kk_r[h].next(); lf_ = lf_r[h].next(); v_ = v_r[h].next(); zs_ = zs_r[h].next()
                        fw.dma("sp", qT_[:], fm_bf[FMSI["hq%d" % h], :, ts], reads=[fm_bf], writes=[qT_])
                        fw.dma("sp", kk_[:], fm_bf[FMSI["kk%d" % h], :, ts], reads=[fm_bf], writes=[kk_])
                        fw.dma("sp", lf_[:], fm_lf[h, :, ts], reads=[fm_lf], writes=[lf_])
                        fw.dma("sp", v_[:], tm_bf[ts, h * 128:(h + 1) * 128].rearrange("(c s) d -> s c d", s=64), reads=[tm_bf], writes=[v_])
                        fw.dma("sp", zs_[:], tm_bf[ts, 256 + h * 128:256 + (h + 1) * 128].rearrange("(c s) d -> s c d", s=64), reads=[tm_bf], writes=[zs_])
                        bT = bT_r[h].next(); d1 = d1_r[h].next(); d4 = d4_r[h].next()
                        e1 = e1_r[h].next(); e2 = e2_r[h].next(); e3 = e3_r[h].next(); e4 = e4_r[h].next()
                        qe = qe_r[h].next(); ke = ke_r[h].next(); qb = qb_r[h].next(); kd = kd_r[h].next()
                        fw.op("dve", lambda e: e.tensor_tensor_scan(out=bT[:], data0=segm_sb[:], data1=lf_[:], initial=0.0, op0=ALU.mult, op1=ALU.add),
                              reads=[segm_sb, lf_], writes=[bT])
                        b3 = bT[:].rearrange("p (c s) -> p c s", s=64)
                        fw.op("dve", lambda e: e.tensor_tensor(out=d1[:].rearrange("p (c s) -> p c s", s=64), in0=b3,
                                                               in1=b3[:, :, 31:32].to_broadcast([128, 8, 64]), op=ALU.subtract), reads=[bT], writes=[d1])
                        fw.op("pool", lambda e: e.tensor_tensor(out=d4[:].rearrange("p (c s) -> p c s", s=64), in0=b3[:, :, 63:64].to_broadcast([128, 8, 64]),
                                                                in1=b3, op=ALU.subtract), reads=[bT], writes=[d4])
                        fw.op("act", lambda e: e.activation(out=e1[:], in_=d1[:], func=AF.Exp), reads=[d1], writes=[e1])
                        fw.op("act", lambda e: e.activation(out=e2[:], in_=d1[:], func=AF.Exp, scale=-1.0), reads=[d1], writes=[e2])
                        fw.op("act", lambda e: e.activation(out=e3[:], in_=bT[:], func=AF.Exp), reads=[bT], writes=[e3])
                        fw.op("act", lambda e: e.activation(out=e4[:], in_=d4[:], func=AF.Exp), reads=[d4], writes=[e4])
                        fw.op("dve", lambda e: e.tensor_tensor(out=qe[:], in0=qT_[:], in1=e1[:], op=ALU.mult), reads=[qT_, e1], writes=[qe])
                        fw.op("pool", lambda e: e.tensor_tensor(out=ke[:], in0=kk_[:], in1=e2[:], op=ALU.mult), reads=[kk_, e2], writes=[ke])
                        fw.op("dve", lambda e: e.tensor_tensor(out=qb[:], in0=qT_[:], in1=e3[:], op=ALU.mult), reads=[qT_, e3], writes=[qb])
                        fw.op("pool", lambda e: e.tensor_tensor(out=kd[:], in0=kk_[:], in1=e4[:], op=ALU.mult), reads=[kk_, e4], writes=[kd])
                        T.append(dict(v=v_, zs=zs_, e3=e3, qe=qe, ke=ke, qb=qb, kd=kd, osb=osb_r[h].next(), ofin=ofin_r[h].next()))
                    for c in range(8):
                        cs_ = slice(c * 64, (c + 1) * 64)
                        for h in range(2):
                            t_ = T[h]
                            ptr = ps_tr.next()
                            fw.op("pe", lambda e: e.transpose(out=ptr[:], in_=t_["kd"][:, cs_], identity=ident[:]), reads=[t_["kd"], ident], writes=[ptr])
                            kdtm = kdtm_r[h].next()
                            fw.op("act", lambda e: e.activation(out=kdtm[:], in_=ptr[:], func=AF.Copy), reads=[ptr], writes=[kdtm])
                            pat = ps_at.next()
                            fw.op("pe", lambda e: e.matmul(pat[:], lhsT=t_["ke"][:, cs_], rhs=t_["qe"][:, cs_], start=True, stop=True),
                                  reads=[t_["ke"], t_["qe"]], writes=[pat])
                            AT = AT_r[h].next()
                            fw.op("dve", lambda e: e.tensor_tensor(out=AT[:], in0=pat[:], in1=cm_sb[:], op=ALU.mult), reads=[pat, cm_sb], writes=[AT])
                            po = ps_o.next()
                            fw.op("pe", lambda e: e.matmul(po[:], lhsT=AT[:], rhs=t_["v"][:, c, :], start=True, stop=False),
                                  reads=[AT, t_["v"]], writes=[po], signal=False)
                            fw.op("pe", lambda e: e.matmul(po[:], lhsT=t_["qb"][:, cs_], rhs=state_bf[h][:], start=False, stop=True),
                                  reads=[t_["qb"], state_bf[h]], writes=[po])
                            pu = ps_u.next()
                            fw.op("pe", lambda e: e.matmul(pu[:], lhsT=kdtm[:], rhs=t_["v"][:, c, :], start=True, stop=True),
                                  reads=[kdtm, t_["v"]], writes=[pu])
                            fw.op("dve", lambda e: e.scalar_tensor_tensor(out=state[h][:], in0=state[h][:], scalar=t_["e3"][:, c * 64 + 63:c * 64 + 64],
                                                                          in1=pu[:], op0=ALU.mult, op1=ALU.add),
                                  reads=[state[h], t_["e3"], pu], writes=[state[h]])
                            fw.op("act", lambda e: e.activation(out=state_bf[h][:], in_=state[h][:], func=AF.Copy), reads=[state[h]], writes=[state_bf[h]])
                            fw.op("act", lambda e: e.activation(out=t_["osb"][:, c, :], in_=po[:], func=AF.Copy), reads=[po], writes=[t_["osb"]])
                    for h in range(2):
                        t_ = T[h]
                        osb = t_["osb"]; ofin = t_["ofin"]
                        fw.op("pool", lambda e: e.tensor_tensor(out=sqt[:], in0=osb[:], in1=osb[:], op=ALU.mult), reads=[osb], writes=[sqt])
                        fw.op("dve", lambda e: e.tensor_reduce(out=ssq[:], in_=sqt[:], axis=AX.X, op=ALU.add), reads=[sqt], writes=[ssq])
                        fw.op("dve", lambda e: e.tensor_scalar(out=ssq[:], in0=ssq[:], scalar1=1.0 / 128, scalar2=EPS, op0=ALU.mult, op1=ALU.add),
                              reads=[ssq], writes=[ssq])
                        fw.op("act", lambda e: e.activation(out=ssq[:], in_=ssq[:], func=AF.Sqrt), reads=[ssq], writes=[ssq])
                        fw.op("dve", lambda e: e.reciprocal(out=ssq[:], in_=ssq[:]), reads=[ssq], writes=[ssq])
                        fw.op("dve", lambda e: e.tensor_tensor(out=ofin[:], in0=osb[:], in1=ssq[:].unsqueeze(2).to_broadcast([64, 8, 128]), op=ALU.mult),
                              reads=[osb, ssq], writes=[ofin])
                        fw.op("pool", lambda e: e.tensor_tensor(out=ofin[:], in0=ofin[:], in1=gh_sb[0:64, :].unsqueeze(1).to_broadcast([64, 8, 128]), op=ALU.mult),
                              reads=[ofin, gh_sb], writes=[ofin])
                        fw.op("pool", lambda e: e.tensor_tensor(out=ofin[:], in0=ofin[:], in1=t_["zs"][:], op=ALU.mult), reads=[ofin, t_["zs"]], writes=[ofin])
                        fw.dma("pool", o_out[ts, h * 128:(h + 1) * 128].rearrange("(c s) d -> s c d", s=64), ofin[:], reads=[ofin], writes=[o_out])
                fw.barrier()
            fw.es = es

        if 3 in phases:
            es34 = ExitStack()
            es.enter_context(es34)
            fw.es = es34
            kcmpT = fw.sb([128, NCP], BF16, "kcmpT")
            cmpR = fw.sb([128, NCT, 385], BF16, "cmpR")
            with ExitStack() as es3:
                fw.es = es3
                big = fw.sb([128, S], BF16, "p3big")
                stg = Rot([fw.sb([128, 2048], F32, "p3stg%d" % i) for i in range(2)])
                w1_sb = fw.sb([128, 32, 128], BF16, "p3w1")
                pos_f = fw.sb([128, 32], F32, "p3posf"); pos_b = fw.sb([128, 32], BF16, "p3posb")
                b1_sb = fw.sb([128, 1], F32, "p3b1"); cb = fw.sb([128, 1], F32, "p3cb")
                w2_f = fw.sb([128, 256], F32, "p3w2f"); w2_b = fw.sb([128, 256], BF16, "p3w2b")
                h1T = fw.sb([128, NCP], BF16, "p3h1T")
                csC = fw.sb([128, 2, NCP], F32, "p3csC")
                t1 = fw.sb([128, 512], F32, "p3t1"); t2 = fw.sb([128, 512], F32, "p3t2")
                pps = Rot([fw.ps([128, 512], F32, "p3ps%d" % i) for i in range(4)])
                fw.dma("sp", csC[:, 0, :], cosC[:, :], reads=[cosC], writes=[csC])
                fw.dma("sp", csC[:, 1, :], sinC[:, :], reads=[sinC], writes=[csC])
                for nt in range(NCT):
                    st = stg.next()
                    fw.dma("sp", st[:, 0:257], ovl[:, nt * 257:(nt + 1) * 257], reads=[ovl], writes=[st])
                    fw.op("dve", lambda e: e.tensor_copy(out=cmpR[:, nt, 0:257], in_=st[:, 0:257]), reads=[st], writes=[cmpR])
                for which in ("k", "v"):
                    w1d, posd, b1d, w2d = (w1k, posk, b1k, w2k) if which == "k" else (w1v, posv, b1v, w2v)
                    w2c = 256 if which == "k" else 128
                    for c0 in range(0, S, 4096):
                        cw = min(4096, S - c0)
                        fw.dma("sp", big[:, c0:c0 + cw], fm_bf[FMSI["kc" if which == "k" else "vc"], :, c0:c0 + cw], reads=[fm_bf], writes=[big])
                    for c0 in (0, 2048):
                        st = stg.next()
                        fw.dma("sp", st[:], w1d[:, c0:c0 + 2048], reads=[w1d], writes=[st])
                        fw.op("act", lambda e: e.activation(out=w1_sb[:].rearrange("p l j -> p (l j)")[:, c0:c0 + 2048], in_=st[:], func=AF.Copy),
                              reads=[st], writes=[w1_sb])
                    fw.dma("sp", pos_f[:], posd[:, :], reads=[posd], writes=[pos_f])
                    fw.op("dve", lambda e: e.tensor_copy(out=pos_b[:], in_=pos_f[:]), reads=[pos_f], writes=[pos_b])
                    fw.dma("sp", b1_sb[:], b1d[:, :], reads=[b1d], writes=[b1_sb])
                    fw.dma("sp", w2_f[:, 0:w2c], w2d[:, :], reads=[w2d], writes=[w2_f])
                    fw.op("dve", lambda e: e.tensor_copy(out=w2_b[:, 0:w2c], in_=w2_f[:, 0:w2c]), reads=[w2_f], writes=[w2_b])
                    pc = pps.next()
                    for l in range(32):
                        fw.op("pe", lambda e, l=l: e.matmul(pc[:, 0:1], lhsT=w1_sb[:, l, :], rhs=pos_b[:, l:l + 1], start=(l == 0), stop=(l == 31)),
                              reads=[w1_sb, pos_b], writes=[pc], signal=(l == 31))
                    fw.op("dve", lambda e: e.tensor_tensor(out=cb[:], in0=pc[:, 0:1], in1=b1_sb[:], op=ALU.add), reads=[pc, b1_sb], writes=[cb])
                    fw.op("pool", lambda e: e.memset(h1T[:], 0.0), writes=[h1T])
                    big3 = big[:].rearrange("p (n s) -> p n s", s=16)
                    for n0 in range(0, NCMP, 512):
                        nn = min(512, NCMP - n0)
                        pp = pps.next()
                        for l in range(32):
                            fw.op("pe", lambda e, l=l: e.matmul(pp[:, 0:nn], lhsT=w1_sb[:, l, :], rhs=big3[:, n0 + l // 16:n0 + l // 16 + nn, l % 16],
                                                                 start=(l == 0), stop=(l == 31)),
                                  reads=[w1_sb, big], writes=[pp], signal=(l == 31))
                        fw.op("act", lambda e: e.activation(out=h1T[:, n0:n0 + nn], in_=pp[:, 0:nn], func=AF.Silu, bias=cb[:, 0:1]),
                              reads=[pp, cb], writes=[h1T])
                    if which == "k":
                        fw.op("pool", lambda e: e.memset(kcmpT[:], 0.0), writes=[kcmpT])
                        for n0 in range(0, NCMP, 512):
                            nn = min(512, NCMP - n0)
                            p1_ = pps.next(); p2_ = pps.next()
                            fw.op("pe", lambda e: e.matmul(p1_[:, 0:nn], lhsT=w2_b[:, 0:128], rhs=h1T[:, n0:n0 + nn], start=True, stop=True),
                                  reads=[w2_b, h1T], writes=[p1_])
                            fw.op("pe", lambda e: e.matmul(p2_[:, 0:nn], lhsT=w2_b[:, 128:256], rhs=h1T[:, n0:n0 + nn], start=True, stop=True),
                                  reads=[w2_b, h1T], writes=[p2_])
                            fw.op("dve", lambda e: e.tensor_tensor(out=t1[:, 0:nn], in0=p1_[:, 0:nn], in1=csC[:, 0, n0:n0 + nn], op=ALU.mult),
                                  reads=[p1_, csC], writes=[t1])
                            fw.op("dve", lambda e: e.tensor_tensor(out=t2[:, 0:nn], in0=p2_[:, 0:nn], in1=csC[:, 1, n0:n0 + nn], op=ALU.mult),
                                  reads=[p2_, csC], writes=[t2])
                            fw.op("pool", lambda e: e.tensor_tensor(out=kcmpT[:, n0:n0 + nn], in0=t1[:, 0:nn], in1=t2[:, 0:nn], op=ALU.add),
                                  reads=[t1, t2], writes=[kcmpT])
                    else:
                        for nt in range(NCT):
                            pv = pps.next()
                            fw.op("pe", lambda e: e.matmul(pv[:, 0:128], lhsT=h1T[:, nt * 128:(nt + 1) * 128], rhs=w2_b[:, 0:128], start=True, stop=True),
                                  reads=[h1T, w2_b], writes=[pv])
                            fw.op("act", lambda e: e.activation(out=cmpR[:, nt, 257:385], in_=pv[:, 0:128], func=AF.Copy), reads=[pv], writes=[cmpR])
                fw.barrier()
            fw.es = es34

        if 4 in phases:
            with ExitStack() as es4:
                fw.es = es4
                ksT = fw.sb([128, S], BF16, "ksT"); kwT = fw.sb([128, S], BF16, "kwT")
                vsA = fw.sb([128, NQT, 129], BF16, "vsA"); vwA = fw.sb([128, NQT, 129], BF16, "vwA")
                for c0 in range(0, S, 4096):
                    cw = min(4096, S - c0)
                    fw.dma("sp", ksT[:, c0:c0 + cw], fm_bf[FMSI["ks"], :, c0:c0 + cw], reads=[fm_bf], writes=[ksT])
                    fw.dma("sp", kwT[:, c0:c0 + cw], fm_bf[FMSI["kw"], :, c0:c0 + cw], reads=[fm_bf], writes=[kwT])
                fw.op("pool", lambda e: e.memset(vsA[:], 1.0), writes=[vsA])
                fw.op("pool", lambda e: e.memset(vwA[:], 1.0), writes=[vwA])
                for k0 in range(0, NQT, 8):
                    kn = min(8, NQT - k0)
                    fw.dma("sp", vsA[:, k0:k0 + kn, 0:128], tm_bf[k0 * 128:(k0 + kn) * 128, 512:640].rearrange("(kt p) d -> p kt d", p=128),
                           reads=[tm_bf], writes=[vsA])
                    fw.dma("sp", vwA[:, k0:k0 + kn, 0:128], tm_bf[k0 * 128:(k0 + kn) * 128, 640:768].rearrange("(kt p) d -> p kt d", p=128),
                           reads=[tm_bf], writes=[vwA])
                e16_sb = fw.sb([128, 8192], BF16, "e16b")
                e16_st = Rot([fw.sb([128, 1024], F32, "e16f%d" % i) for i in range(2)])
                for c0 in range(0, 8192, 1024):
                    st = e16_st.next()
                    fw.dma("sp", st[:], e16[:, c0:c0 + 1024], reads=[e16], writes=[st])
                    fw.op("dve", lambda e: e.tensor_copy(out=e16_sb[:, c0:c0 + 1024], in_=st[:]), reads=[st], writes=[e16_sb])
                qT4_r = Rot([fw.sb([128, 4, 128], BF16, "qT4_%d" % i) for i in range(2)])
                ng_r = Rot([fw.sb([128, 6], F32, "ngt_%d" % i) for i in range(2)])
                nz_r = Rot([fw.sb([128, 2, 128], BF16, "nzt_%d" % i) for i in range(2)])
                pT_r = Rot([fw.sb([128, 512], BF16, "pT_%d" % i) for i in range(3)])
                rc = fw.sb([128, 4], F32, "rc"); coef = fw.sb([128, 6], F32, "coef"); zr = fw.sb([128, 4], F32, "zr")
                imp = fw.sb([128, 256], F32, "imp"); imp2 = fw.sb([128, 256], F32, "imp2"); tmpk = fw.sb([128, 256], F32, "tmpk")
                m8a = fw.sb([128, 8], F32, "m8a"); m8b = fw.sb([128, 8], F32, "m8b")
                mask = fw.sb([128, 256], BF16, "mask"); maskT = [fw.sb([128, 128], BF16, "maskT%d" % i) for i in range(2)]
                oacc = fw.sb([128, 2, 128], F32, "oacc")
                ofin_r = Rot([fw.sb([128, 2, 128], F32, "p4ofin%d" % i) for i in range(2)])
                psS = Rot([fw.ps([128, 512], F32, "psS%d" % i) for i in range(2)])
                accB = [fw.ps([128, 512], F32, "accB%d" % i) for i in range(4)]
                psM_t = fw.ps([128, 512], F32, "psM")
                psM = Rot([Buf(psM_t[:, i * 128:(i + 1) * 128], "psM%d" % i) for i in range(4)])
                psT_t = fw.ps([128, 1024], BF16, "psT")
                psT = Rot([Buf(psT_t[:, i * 128:(i + 1) * 128], "psT%d" % i) for i in range(4)])
                import os as _os
                for qt in range(int(_os.environ.get('P4_QTMIN', '0')), min(NQT, int(_os.environ.get('P4_QTMAX', '100000')))):
                    tq = slice(qt * 128, (qt + 1) * 128)
                    qT4 = qT4_r.next(); ngt = ng_r.next(); nzt = nz_r.next()
                    fw.dma("sp", qT4[:], fm_bf[FMSI["nq0"]:FMSI["nq0"] + 4, :, tq].rearrange("h p t -> p h t"), reads=[fm_bf], writes=[qT4])
                    fw.dma("sp", ngt[:], tm_ng[tq, :], reads=[tm_ng], writes=[ngt])
                    fw.dma("sp", nzt[:], tm_bf[tq, 768:1024].rearrange("p (h d) -> p h d", h=2), reads=[tm_bf], writes=[nzt])
                    q4f = qT4[:].rearrange("p h t -> p (h t)")
                    NTq = min(NCT, (8 * qt + 6) // 128 + 1)
                    for nt in range(NTq):
                        sps = psS.next()
                        fw.op("pe", lambda e: e.matmul(sps[:], lhsT=kcmpT[:, nt * 128:(nt + 1) * 128], rhs=q4f, start=True, stop=True),
                              reads=[kcmpT, qT4], writes=[sps])
                        pT = pT_r.next()
                        fw.op("act", lambda e: e.activation(out=pT[:], in_=sps[:], func=AF.Exp, scale=SCALE), reads=[sps], writes=[pT])
                        if nt >= NTq - 2 and 'm' not in _os.environ.get('P4_SKIP', ''):
                            fw.op("pool", lambda e: e.affine_select(out=pT[:].rearrange("p (h t) -> p h t", h=4), in_=pT[:].rearrange("p (h t) -> p h t", h=4),
                                                                    pattern=[[0, 4], [1, 128]], compare_op=ALU.is_ge, fill=FILL0,
                                                                    base=128 * qt - 16 * nt * 128 - 31, channel_multiplier=-16),
                                  reads=[pT], writes=[pT])
                        for h in range(4):
                            fw.op("pe", lambda e, h=h: e.matmul(accB[h][:, 0:385], lhsT=pT[:, h * 128:(h + 1) * 128], rhs=cmpR[:, nt, :],
                                                                 start=(nt == 0), stop=(nt == NTq - 1)),
                                  reads=[pT, cmpR], writes=[accB[h]], signal=(nt == NTq - 1))
                    for h in range(4):
                        fw.op("dve", lambda e, h=h: e.tensor_scalar(out=zr[:, h:h + 1], in0=accB[h][:, 256:257], scalar1=1e-30, scalar2=None, op0=ALU.max),
                              reads=[accB[h]], writes=[zr])
                    fw.op("dve", lambda e: e.reciprocal(out=rc[:], in_=zr[:]), reads=[zr], writes=[rc])
                    fw.op("dve", lambda e: e.tensor_scalar(out=imp[:], in0=accB[0][:, 0:256], scalar1=rc[:, 0:1], scalar2=None, op0=ALU.mult),
                          reads=[accB[0], rc], writes=[imp])
                    for h in range(1, 4):
                        fw.op("dve", lambda e, h=h: e.scalar_tensor_tensor(out=imp[:], in0=accB[h][:, 0:256], scalar=rc[:, h:h + 1], in1=imp[:],
                                                                           op0=ALU.mult, op1=ALU.add), reads=[accB[h], rc, imp], writes=[imp])
                    for i in range(2):
                        fw.op("dve", lambda e, i=i: e.tensor_tensor(out=coef[:, 3 * i:3 * i + 1], in0=rc[:, i:i + 1], in1=ngt[:, 3 * i:3 * i + 1], op=ALU.mult),
                              reads=[rc, ngt], writes=[coef])
                    for i in range(2):
                        fw.op("dve", lambda e, i=i: e.tensor_scalar(out=oacc[:, i, :], in0=accB[i][:, 257:385], scalar1=coef[:, 3 * i:3 * i + 1], scalar2=None,
                                                                    op0=ALU.mult), reads=[accB[i], coef], writes=[oacc])
                    if 'k' in _os.environ.get('P4_SKIP', ''):
                        continue
                    fw.op("pool", lambda e: e.affine_select(out=imp2[:], in_=imp[:], pattern=[[-64, 256]], compare_op=ALU.is_ge, fill=FILLF,
                                                            base=128 * qt - 128, channel_multiplier=1), reads=[imp], writes=[imp2])
                    fw.op("pool", lambda e: e.affine_select(out=imp2[:], in_=imp2[:], pattern=[[-64, 256]], compare_op=ALU.is_ge, fill=FILLM,
                                                            base=128 * qt, channel_multiplier=1), reads=[imp2], writes=[imp2])
                    fw.op("pool", lambda e: e.memset(imp2[:, 0:1], 1.0e4), reads=[imp2], writes=[imp2])
                    fw.op("dve", lambda e: e.max(out=m8a[:], in_=imp2[:]), reads=[imp2], writes=[m8a])
                    fw.op("dve", lambda e: e.match_replace(out=tmpk[:], in_to_replace=m8a[:], in_values=imp2[:], imm_value=NEG), reads=[imp2, m8a], writes=[tmpk])
                    fw.op("dve", lambda e: e.max(out=m8b[:], in_=tmpk[:]), reads=[tmpk], writes=[m8b])
                    fw.op("dve", lambda e: e.tensor_scalar(out=mask[:], in0=imp2[:], scalar1=m8b[:, 7:8], scalar2=None, op0=ALU.is_ge),
                          reads=[imp2, m8b], writes=[mask])
                    for gq in range((2 * qt + 1) // 128 + 1):
                        ptr = psT.next()
                        fw.op("pe", lambda e: e.transpose(out=ptr[:], in_=mask[:, gq * 128:(gq + 1) * 128], identity=ident[:]), reads=[mask, ident], writes=[ptr])
                        fw.op("act", lambda e: e.activation(out=maskT[gq][:], in_=ptr[:], func=AF.Copy), reads=[ptr], writes=[maskT[gq]])
                    for br in [int(c) for c in _os.environ.get('P4_BR', '12')]:
                        KT_, VA_ = (ksT, vsA) if br == 1 else (kwT, vwA)
                        acc = accB[1 + br]
                        kts = list(range(0, min(qt + 1, int(_os.environ.get('P4_KTMAX', '100000'))))) if br == 1 else list(range(max(0, qt - 4), qt + 1))
                        for kt in kts:
                            sps = psS.next()
                            fw.op("pe", lambda e: e.matmul(sps[:, 0:256], lhsT=KT_[:, kt * 128:(kt + 1) * 128], rhs=q4f[:, 0:256], start=True, stop=True),
                                  reads=[KT_, qT4], writes=[sps])
                            pT = pT_r.next()
                            fw.op("act", lambda e: e.activation(out=pT[:, 0:256], in_=sps[:, 0:256], func=AF.Exp, scale=SCALE), reads=[sps], writes=[pT])
                            p3 = pT[:, 0:256].rearrange("p (h t) -> p h t", h=2)
                            if br == 1:
                                pm = psM.next()
                                fw.op("pe", lambda e: e.matmul(pm[:], lhsT=e16_sb[:, (kt % 64) * 128:(kt % 64 + 1) * 128], rhs=maskT[kt // 64][:],
                                                               start=True, stop=True), reads=[e16_sb, maskT[kt // 64]], writes=[pm])
                                fw.op("dve", lambda e: e.tensor_tensor(out=p3, in0=p3, in1=pm[:].unsqueeze(1).to_broadcast([128, 2, 128]), op=ALU.mult),
                                      reads=[pT, pm], writes=[pT])
                            if kt == qt:
                                fw.op("pool", lambda e: e.affine_select(out=p3, in_=p3, pattern=[[0, 2], [1, 128]], compare_op=ALU.is_ge, fill=FILL0,
                                                                        base=0, channel_multiplier=-1), reads=[pT], writes=[pT])
                            if br == 2 and kt == qt - 4:
                                fw.op("pool", lambda e: e.affine_select(out=p3, in_=p3, pattern=[[0, 2], [-1, 128]], compare_op=ALU.is_gt, fill=FILL0,
                                                                        base=0, channel_multiplier=1), reads=[pT], writes=[pT])
                            for i in range(2):
                                fw.op("pe", lambda e, i=i: e.matmul(acc[:, i * 129:(i + 1) * 129], lhsT=pT[:, i * 128:(i + 1) * 128], rhs=VA_[:, kt, :],
                                                                     start=(kt == kts[0] and i == 0), stop=(kt == kts[-1]), skip_group_check=True),
                                      reads=[pT, VA_], writes=[acc], signal=(kt == kts[-1] and i == 1))
                        for i in range(2):
                            fw.op("dve", lambda e, i=i: e.reciprocal(out=zr[:, i:i + 1], in_=acc[:, i * 129 + 128:i * 129 + 129]), reads=[acc], writes=[zr])
                            fw.op("dve", lambda e, i=i: e.tensor_tensor(out=coef[:, 3 * i + br:3 * i + br + 1], in0=zr[:, i:i + 1], in1=ngt[:, 3 * i + br:3 * i + br + 1],
                                                                        op=ALU.mult), reads=[zr, ngt], writes=[coef])
                            fw.op("dve", lambda e, i=i: e.scalar_tensor_tensor(out=oacc[:, i, :], in0=acc[:, i * 129:i * 129 + 128], scalar=coef[:, 3 * i + br:3 * i + br + 1],
                                                                               in1=oacc[:, i, :], op0=ALU.mult, op1=ALU.add), reads=[acc, coef, oacc], writes=[oacc])
                    ofin = ofin_r.next()
                    fw.op("pool", lambda e: e.tensor_tensor(out=ofin[:], in0=oacc[:], in1=nzt[:], op=ALU.mult), reads=[oacc, nzt], writes=[ofin])
                    fw.dma("pool", o_out[tq, 256:512].rearrange("p (h d) -> p h d", h=2), ofin[:], reads=[ofin], writes=[o_out])
                fw.barrier()
            fw.es = es

        outs_to_wait = []
        for nme, dstb in dbg.items():
            srcb = {"fm_bf": fm_bf, "fm_lf": fm_lf, "tm_bf": tm_bf, "tm_ng": tm_ng}[nme]
            if len(srcb.t.shape) == 3:
                for q in range(srcb.t.shape[0]):
                    fw.dma("sp", dstb[q, :, :], srcb[q, :, :], reads=[srcb], writes=[dstb])
            else:
                fw.dma("sp", dstb[:, :], srcb[:, :], reads=[srcb], writes=[dstb])
            outs_to_wait.append(dstb)
        fw.finish(outs_to_wait + [o_out], e="sp")
        print("instr counts", fw.cnt, "dma", sum(fw.dcnt) // 16)
    return nc


OFF = dict(hq=0, hf=1024, hi=2048, hz=3072, nq=4096, kc=5120, vc=5376, ks=5632, vs=5888, kw=6144, vw=6400, ng=6656, nz=6680)
_SWAP = np.concatenate([np.arange(16, 32), np.arange(0, 16), np.arange(32, 128)])


def rope_tables(pos):
    inv = (np.float32(ROPE_THETA) ** (-2.0 * np.arange(16, dtype=np.float32) / np.float32(32))).astype(np.float32)
    ang = (pos.astype(np.float32)[None, :] * inv[:, None]).astype(np.float32).astype(np.float64)
    L = pos.shape[0]
    c = np.ones((128, L), np.float32)
    s = np.zeros((128, L), np.float32)
    c[0:16] = np.cos(ang); c[16:32] = np.cos(ang)
    s[0:16] = -np.sin(ang); s[16:32] = np.sin(ang)
    return c, s


def mixer_consts(S):
    NCMP = S // 16 - 1
    NCT = (NCMP + 127) // 128
    NCP = NCT * 128
    cT, sT = rope_tables(np.arange(S))
    cC, sC = rope_tables(np.arange(NCP) * 16 + 31)
    nsel = S // 64
    n = np.arange(NCP)[:, None]
    j = np.arange(256)[None, :]
    ov = ((n * 16 < j * 64 + 64) & (n * 16 + 32 > j * 64) & (n < NCMP) & (j < nsel)).astype(np.float32)
    ovl = np.zeros((NCP, 257), np.float32)
    ovl[:, :256] = ov
    ovl[:NCMP, 256] = 1.0
    ovl = ovl.reshape(NCT, 128, 257).transpose(1, 0, 2).reshape(128, NCT * 257)
    e16 = np.zeros((128, 8192), np.float32)
    kk = np.arange(8192)
    e16[kk // 64, kk] = 1.0
    segm = np.ones((128, 512), np.float32)
    segm[:, ::64] = 0.0
    cm64 = (np.arange(64)[:, None] <= np.arange(64)[None, :]).astype(np.float32)
    return dict(cosT=cT, sinT=sT, cosC=cC, sinC=sC, ovl=np.ascontiguousarray(ovl), e16=e16, segm=segm, cm64=cm64,
                ident=np.eye(128, dtype=np.float32))


def mixer_inputs(core, layer, xT_b, P, consts):
    r = core % 4
    g = r // 2
    hp = r % 2
    w = P["w_in"][layer]
    hh = [2 * r, 2 * r + 1]
    own = [g * 4 + hp * 2, g * 4 + hp * 2 + 1]
    qh = own + [g * 4 + (1 - hp) * 2, g * 4 + (1 - hp) * 2 + 1]

    def blk(name, h):
        return w[:, OFF[name] + h * 128: OFF[name] + (h + 1) * 128]

    cols = {"hq0": blk("hq", hh[0]), "hq1": blk("hq", hh[1]), "hf0": blk("hf", hh[0]), "hf1": blk("hf", hh[1]),
            "kc": blk("kc", g), "vc": blk("vc", g), "ks": blk("ks", g), "kw": blk("kw", g)}
    for i in range(4):
        cols["nq%d" % i] = blk("nq", qh[i])
    for nme in ("nq0", "nq1", "nq2", "nq3", "ks", "kw"):
        cols[nme + "s"] = cols[nme][:, _SWAP]
    wfm = np.concatenate([cols[nme] for nme in FM_BLOCKS], axis=1)
    ngc = np.concatenate([w[:, OFF["ng"] + H * 3: OFF["ng"] + H * 3 + 3] for H in own], axis=1)
    wtm = np.concatenate([blk("hi", hh[0]), blk("hi", hh[1]), blk("hz", hh[0]), blk("hz", hh[1]),
                          blk("vs", g), blk("vw", g), blk("nz", own[0]), blk("nz", own[1]), ngc], axis=1)
    lbl = np.zeros((128, 5), np.float32)
    L = P["hgrn_lb_logits"]
    for i, h in enumerate(hh):
        lbl[:, 2 * i] = L[0, h * 128:(h + 1) * 128]
        lbl[:, 2 * i + 1] = L[1, h * 128:(h + 1) * 128]
    lbl[:, 4] = float(layer)
    d = dict(consts)
    d.update(
        xT=np.ascontiguousarray(xT_b),
        gpre=np.ascontiguousarray(P["pre_norm"][layer].reshape(8, 128).T),
        wfm=np.ascontiguousarray(wfm), wtm=np.ascontiguousarray(wtm), lbl=lbl,
        gh=np.ascontiguousarray(np.broadcast_to(P["hgrn_out_norm"][layer][None, :], (128, 128))),
        w1k=np.ascontiguousarray(P["cmp_w1_k"][layer].reshape(32, 128, 128).transpose(1, 0, 2).reshape(128, 4096)),
        w1v=np.ascontiguousarray(P["cmp_w1_v"][layer].reshape(32, 128, 128).transpose(1, 0, 2).reshape(128, 4096)),
        posk=np.ascontiguousarray(P["cmp_pos_k"][layer].T), posv=np.ascontiguousarray(P["cmp_pos_v"][layer].T),
        b1k=np.ascontiguousarray(P["cmp_b1_k"][layer].reshape(128, 1)), b1v=np.ascontiguousarray(P["cmp_b1_v"][layer].reshape(128, 1)),
        w2k=np.ascontiguousarray(np.concatenate([P["cmp_w2_k"][layer], P["cmp_w2_k"][layer][:, _SWAP]], axis=1)),
        w2v=np.ascontiguousarray(P["cmp_w2_v"][layer]),
    )
    return d


def build_outproj(T):
    nc = bass.Bass("TRN2", target_bir_lowering=False)

    def din(name, shape, dt=F32):
        return Buf(nc.dram_tensor(name, list(shape), dt, kind="ExternalInput").ap(), name)

    oT = din("oT", [2048, T])
    xin = din("xin", [T, 1024])
    wout = din("wout", [2048, 1024])
    gpost = din("gpost", [128, 1024])
    xo = Buf(nc.dram_tensor("xo", [T, 1024], F32, kind="ExternalOutput").ap(), "xo")
    with ExitStack() as es:
        fw = FW(nc, es)
        w_sb = fw.sb([128, 16, 1024], BF16, "w_sb")
        wst = Rot([fw.sb([128, 1024], F32, "wst%d" % i) for i in range(2)])
        wv = wout[:, :].rearrange("(k p) c -> p k c", p=128)
        for k in range(16):
            st = wst.next()
            fw.dma("sp", st[:], wv[:, k, :], reads=[wout], writes=[st])
            if k % 2 == 0:
                fw.op("act", lambda e: e.activation(out=w_sb[:, k, :], in_=st[:], func=AF.Copy), reads=[st], writes=[w_sb])
            else:
                fw.op("dve", lambda e: e.tensor_copy(out=w_sb[:, k, :], in_=st[:]), reads=[st], writes=[w_sb])
        g_sb = fw.sb([128, 1024], F32, "g_sb")
        fw.dma("sp", g_sb[:], gpost[:, :], reads=[gpost], writes=[g_sb])
        of_r = Rot([fw.sb([128, 16, 128], F32, "of%d" % i) for i in range(2)])
        ob_r = Rot([fw.sb([128, 16, 128], BF16, "ob%d" % i) for i in range(2)])
        x_r = Rot([fw.sb([128, 1024], F32, "x%d" % i) for i in range(2)])
        r_r = Rot([fw.sb([128, 1024], F32, "r%d" % i) for i in range(2)])
        sq = fw.sb([128, 512], F32, "sq")
        ss = fw.sb([128, 2], F32, "ss")
        rs = fw.sb([128, 1], F32, "rs")
        pss = Rot([fw.ps([128, 512], F32, "ps%d" % i) for i in range(4)])
        oTv = oT[:, :].rearrange("(k p) t -> p k t", p=128)
        for i in range(T // 128):
            tsl = slice(i * 128, (i + 1) * 128)
            of = of_r.next(); ob = ob_r.next(); xt = x_r.next(); rt = r_r.next()
            fw.dma("sp", of[:], oTv[:, :, tsl], reads=[oT], writes=[of])
            fw.dma("sp", xt[:], xin[tsl, :], reads=[xin], writes=[xt])
            fw.op("pool", lambda e: e.tensor_copy(out=ob[:], in_=of[:]), reads=[of], writes=[ob])
            ps2 = [pss.next(), pss.next()]
            for hf in range(2):
                for k in range(16):
                    fw.op("pe", lambda e, k=k, hf=hf: e.matmul(ps2[hf][:], lhsT=ob[:, k, :], rhs=w_sb[:, k, hf * 512:(hf + 1) * 512],
                                                                start=(k == 0), stop=(k == 15)),
                          reads=[ob, w_sb], writes=[ps2[hf]], signal=(k == 15))
                fw.op("act", lambda e, hf=hf: e.activation(out=sq[:], in_=ps2[hf][:], func=AF.Square, accum_out=ss[:, hf:hf + 1]),
                      reads=[ps2[hf]], writes=[sq, ss])
            fw.op("dve", lambda e: e.tensor_tensor(out=rs[:], in0=ss[:, 0:1], in1=ss[:, 1:2], op=ALU.add), reads=[ss], writes=[rs])
            fw.op("dve", lambda e: e.tensor_scalar(out=rs[:], in0=rs[:], scalar1=1.0 / 1024, scalar2=EPS, op0=ALU.mult, op1=ALU.add), reads=[rs], writes=[rs])
            fw.op("act", lambda e: e.activation(out=rs[:], in_=rs[:], func=AF.Sqrt), reads=[rs], writes=[rs])
            fw.op("dve", lambda e: e.reciprocal(out=rs[:], in_=rs[:]), reads=[rs], writes=[rs])
            for hf in range(2):
                cs_ = slice(hf * 512, (hf + 1) * 512)
                fw.op("dve", lambda e, hf=hf, cs_=cs_: e.scalar_tensor_tensor(out=rt[:, cs_], in0=ps2[hf][:], scalar=rs[:, 0:1], in1=g_sb[:, cs_],
                                                                              op0=ALU.mult, op1=ALU.mult), reads=[ps2[hf], rs, g_sb], writes=[rt])
            fw.op("pool", lambda e: e.tensor_tensor(out=rt[:], in0=rt[:], in1=xt[:], op=ALU.add), reads=[rt, xt], writes=[rt])
            fw.dma("pool", xo[tsl, :], rt[:], reads=[rt], writes=[xo])
        fw.finish([xo], e="sp")
    return nc


_CACHE = {}


def _get(name, fn):
    if name not in _CACHE:
        _CACHE[name] = fn()
    return _CACHE[name]


def run_layer(x, layer, P, S, consts):
    ncm = _get(("mixer", S), lambda: build_mixer(S))
    xTs = [np.ascontiguousarray(x[b].T) for b in range(2)]
    in_maps = [mixer_inputs(c, layer, xTs[c // 4], P, consts) for c in range(8)]
    res = run_bass_kernel_spmd(ncm, in_maps, core_ids=list(range(8)))
    o_full = np.zeros((2, S, 2048), np.float32)
    for c in range(8):
        b, r = c // 4, c % 4
        o = np.asarray(res.results[c]["o"])
        o_full[b, :, 2 * r * 128:(2 * r + 2) * 128] = o[:, 0:256]
        o_full[b, :, 1024 + 2 * r * 128:1024 + (2 * r + 2) * 128] = o[:, 256:512]
    T = S // 4
    nco = _get(("outproj", T), lambda: build_outproj(T))
    gp = np.ascontiguousarray(np.broadcast_to(P["post_norm"][layer][None, :], (128, 1024)))
    wo = np.ascontiguousarray(P["w_out"][layer])
    in_maps = []
    for c in range(8):
        b, r = c // 4, c % 4
        in_maps.append(dict(oT=np.ascontiguousarray(o_full[b, r * T:(r + 1) * T].T), xin=np.ascontiguousarray(x[b, r * T:(r + 1) * T]),
                            wout=wo, gpost=gp))
    res = run_bass_kernel_spmd(nco, in_maps, core_ids=list(range(8)))
    x1 = np.zeros_like(x)
    for c in range(8):
        b, r = c // 4, c % 4
        x1[b, r * T:(r + 1) * T] = np.asarray(res.results[c]["xo"])
    return x1


def kernel(**inputs):
    P = {k: np.asarray(v) for k, v in inputs.items()}
    x = np.ascontiguousarray(P["x"], dtype=np.float32)
    S = x.shape[1]
    consts = mixer_consts(S)
    for layer in range(2):
        x = run_layer(x, layer, P, S, consts)
    return x
```
